# Optimizing a Trainium2 kernel written in Bass

```python
import jax
import jax.numpy as jnp
from jax import lax
import numpy as np

D_MODEL = 1024
BATCH = 4
SEQ = 4096
DEPTH = 4

GRID_W = 64
CTX_LEN = 256
N_MIXERS = 2
N_A_LAYERS = (DEPTH + 1) // 2
N_B_LAYERS = DEPTH // 2
N_DENSE_LAYERS = (DEPTH + 1) // 2
N_MOE_LAYERS = DEPTH // 2
EPS = 1e-6
NEG_BIG = -1e30

HG_HEADS = 8
HG_DK = 128
HG_DV = D_MODEL // HG_HEADS
HG_FDIM = HG_HEADS * HG_DK
HG_IN_DIM = 3 * HG_FDIM + 2 * D_MODEL
HG_CHUNK = 32

SW_HEADS = 16
SW_KV_HEADS = 4
SW_GROUP = SW_HEADS // SW_KV_HEADS
SW_HEAD_DIM = 64
SW_Q_DIM = SW_HEADS * SW_HEAD_DIM
SW_KV_DIM = SW_KV_HEADS * SW_HEAD_DIM
SW_WINDOW = 128
SW_BLOCK = 128
ROPE_THETA = 10000.0

D_FF = 2816
N_EXPERTS = 8
TOP_K = 2
D_EXPERT = 3584

kernel_name = 'hybrid_hgrn2_swa_moe_diffusion_trunk'


def rms_norm(x, w=None, eps=EPS):
    xf = x.astype(jnp.float32)
    y = xf * lax.rsqrt(jnp.mean(xf * xf, axis=-1, keepdims=True) + eps)
    if w is not None:
        y = y * w.astype(jnp.float32)
    return y.astype(x.dtype)


def modulate(x, shift, scale):
    return rms_norm(x) * (1.0 + scale) + shift


def axial_rope_tables(n_tokens, head_dim):
    rows = n_tokens // GRID_W
    row = jnp.repeat(jnp.arange(rows, dtype=jnp.float32), GRID_W)
    col = jnp.tile(jnp.arange(GRID_W, dtype=jnp.float32), rows)
    n_freq = head_dim // 4
    inv_freq = ROPE_THETA ** (-jnp.arange(n_freq, dtype=jnp.float32) / n_freq)
    ang = jnp.stack([row, col])[:, :, None] * inv_freq
    return jnp.cos(ang), jnp.sin(ang)


def rotate_half(x, cos, sin):
    x1, x2 = jnp.split(x, 2, axis=-1)
    return jnp.concatenate([x1 * cos - x2 * sin, x2 * cos + x1 * sin], axis=-1)


def apply_axial_rope(x, cos, sin):
    xf = x.astype(jnp.float32)
    half = x.shape[-1] // 2
    y = jnp.concatenate([rotate_half(xf[..., :half], cos[0], sin[0]),
                         rotate_half(xf[..., half:], cos[1], sin[1])], axis=-1)
    return y.astype(x.dtype)


def gla_chunk_scan(q, k, v, log_f, s0):
    b, h, l, dk = q.shape
    dv = v.shape[-1]
    c = HG_CHUNK
    n = l // c
    chunks = lambda t: jnp.moveaxis(t.reshape(b, h, n, c, t.shape[-1]), 2, 0)
    causal = jnp.tril(jnp.ones((c, c), dtype=bool))[:, :, None]

    def step(state, xs):
        q_c, k_c, v_c, lf_c = xs
        a = jnp.cumsum(lf_c, axis=2)
        a_end = a[:, :, -1:, :]
        rel = a[:, :, :, None, :] - a[:, :, None, :, :]
        decay = jnp.exp(jnp.where(causal, rel, NEG_BIG))
        scores = jnp.einsum('bhtk,bhsk,bhtsk->bhts', q_c, k_c, decay)
        o_c = (jnp.einsum('bhts,bhsv->bhtv', scores, v_c)
               + jnp.einsum('bhtk,bhkv->bhtv', q_c * jnp.exp(a), state))
        state = (jnp.exp(a_end[:, :, 0])[..., None] * state
                 + jnp.einsum('bhsk,bhsv->bhkv', k_c * jnp.exp(a_end - a), v_c))
        return state, o_c

    s_final, o = lax.scan(step, s0, (chunks(q), chunks(k), chunks(v), chunks(log_f)))
    return jnp.moveaxis(o, 0, 2).reshape(b, h, l, dv), s_final


def hgrn2_mixer(h_lat, h_ctx, w_in, lower_bound, norm_w, w_out, with_ctx_out):
    dt = h_lat.dtype
    splits = [HG_FDIM, HG_FDIM + D_MODEL, 2 * HG_FDIM + D_MODEL, 3 * HG_FDIM + D_MODEL]

    def project(h):
        b, n, _ = h.shape
        q, v, zf, zb, g = jnp.split((h @ w_in).astype(jnp.float32), splits, axis=-1)
        heads = lambda t, d: t.reshape(b, n, HG_HEADS, d).transpose(0, 2, 1, 3)

        def gate(z, lb):
            f = lb + (1.0 - lb) * jax.nn.sigmoid(z)
            log_f = jnp.log(jnp.maximum(f, 1e-30))
            key = (1.0 - lb) * jax.nn.sigmoid(-z)
            return heads(key, HG_DK), heads(log_f, HG_DK)

        kf, lf = gate(zf, lower_bound[0])
        kb, lb_ = gate(zb, lower_bound[1])
        return heads(jax.nn.silu(q), HG_DK), heads(v, HG_DV), kf, lf, kb, lb_, g

    qc, vc, kfc, lfc, kbc, lbc, gc = project(h_ctx)
    ql, vl, kfl, lfl, kbl, lbl, gl = project(h_lat)
    b = h_lat.shape[0]
    s0 = jnp.zeros((b, HG_HEADS, HG_DK, HG_DV), jnp.float32)
    rev = lambda t: jnp.flip(t, axis=2)
    o_cf, s_cf = gla_chunk_scan(qc, kfc, vc, lfc, s0)
    o_cb, s_cb = gla_chunk_scan(rev(qc), rev(kbc), rev(vc), rev(lbc), s0)
    o_lf, _ = gla_chunk_scan(ql, kfl, vl, lfl, s_cf)
    o_lb, _ = gla_chunk_scan(rev(ql), rev(kbl), rev(vl), rev(lbl), s_cb)

    def readout(o, g):
        bb, _, n, _ = o.shape
        o = rms_norm(o, norm_w).transpose(0, 2, 1, 3).reshape(bb, n, D_MODEL)
        return (o * jax.nn.silu(g)).astype(dt) @ w_out

    y_lat = readout(o_lf + rev(o_lb), gl)
    y_ctx = readout(o_cf + rev(o_cb), gc) if with_ctx_out else None
    return y_lat, y_ctx


def swa_mixer(h_lat, h_ctx, w_qkv, q_norm_w, k_norm_w, sink, w_out, with_ctx_out):
    b, l, _ = h_lat.shape
    dt = h_lat.dtype
    scale = SW_HEAD_DIM ** -0.5

    def project(h):
        n = h.shape[1]
        q, k, v = jnp.split(h @ w_qkv, [SW_Q_DIM, SW_Q_DIM + SW_KV_DIM], axis=-1)
        q = rms_norm(q.reshape(b, n, SW_KV_HEADS, SW_GROUP, SW_HEAD_DIM), q_norm_w).transpose(0, 2, 3, 1, 4)
        k = rms_norm(k.reshape(b, n, SW_KV_HEADS, SW_HEAD_DIM), k_norm_w).transpose(0, 2, 1, 3)
        v = v.reshape(b, n, SW_KV_HEADS, SW_HEAD_DIM).transpose(0, 2, 1, 3)
        return q, k, v

    q, k, v = project(h_lat)
    qc, kc, vc = project(h_ctx)
    n_ctx = kc.shape[2]
    cos, sin = axial_rope_tables(l, SW_HEAD_DIM)
    q = apply_axial_rope(q, cos, sin)
    k = apply_axial_rope(k, cos, sin)
    sink_logit = sink.astype(jnp.float32).reshape(SW_KV_HEADS, SW_GROUP, 1, 1)
    pad = ((0, 0), (0, 0), (SW_BLOCK, SW_BLOCK), (0, 0))
    k_pad = jnp.pad(k, pad)
    v_pad = jnp.pad(v, pad)
    band = 3 * SW_BLOCK

    def attend_block(j):
        start = j * SW_BLOCK
        qb = lax.dynamic_slice_in_dim(q, start, SW_BLOCK, axis=3)
        kb = lax.dynamic_slice_in_dim(k_pad, start, band, axis=2)
        vb = lax.dynamic_slice_in_dim(v_pad, start, band, axis=2)
        q_pos = start + jnp.arange(SW_BLOCK)
        k_pos = start - SW_BLOCK + jnp.arange(band)
        valid = ((jnp.abs(q_pos[:, None] - k_pos[None, :]) <= SW_WINDOW)
                 & (k_pos >= 0)[None, :] & (k_pos < l)[None, :])
        s_loc = jnp.einsum('bkgqd,bksd->bkgqs', qb, kb).astype(jnp.float32) * scale
        s_loc = jnp.where(valid, s_loc, NEG_BIG)
        s_ctx = jnp.einsum('bkgqd,bksd->bkgqs', qb, kc).astype(jnp.float32) * scale
        s_sink = jnp.broadcast_to(sink_logit, s_ctx.shape[:-1] + (1,))
        p = jax.nn.softmax(jnp.concatenate([s_loc, s_ctx, s_sink], axis=-1), axis=-1).astype(dt)
        return (jnp.einsum('bkgqs,bksd->bkgqd', p[..., :band], vb)
                + jnp.einsum('bkgqs,bksd->bkgqd', p[..., band:band + n_ctx], vc))

    o = lax.map(attend_block, jnp.arange(l // SW_BLOCK))
    o = o.transpose(1, 0, 4, 2, 3, 5).reshape(b, l, SW_Q_DIM)
    y_lat = o @ w_out
    y_ctx = None
    if with_ctx_out:
        s = jnp.einsum('bkgqd,bksd->bkgqs', qc, kc).astype(jnp.float32) * scale
        s_sink = jnp.broadcast_to(sink_logit, s.shape[:-1] + (1,))
        p = jax.nn.softmax(jnp.concatenate([s, s_sink], axis=-1), axis=-1).astype(dt)
        oc = jnp.einsum('bkgqs,bksd->bkgqd', p[..., :n_ctx], vc)
        y_ctx = oc.transpose(0, 3, 1, 2, 4).reshape(b, n_ctx, SW_Q_DIM) @ w_out
    return y_lat, y_ctx


def swiglu(h, w_gate_up, w_down):
    g, u = jnp.split(h @ w_gate_up, 2, axis=-1)
    return (jax.nn.silu(g) * u) @ w_down


def moe_swiglu(h, router, w_gate_up, w_down):
    logits = (h @ router).astype(jnp.float32)
    top_logit, top_idx = lax.top_k(logits, TOP_K)
    top_w = jax.nn.softmax(top_logit, axis=-1)
    gates = jnp.sum(jax.nn.one_hot(top_idx, N_EXPERTS, dtype=jnp.float32) * top_w[..., None], axis=-2).astype(h.dtype)
    out = jnp.zeros_like(h)
    for e in range(N_EXPERTS):
        out = out + gates[..., e:e + 1] * swiglu(h, w_gate_up[e], w_down[e])
    return out


def setup_inputs(seed: int = 0) -> dict:
    key = jax.random.key(seed)
    ks = jax.random.split(key, 20)
    d = D_MODEL
    nrm = lambda k, shape, s: jax.random.normal(k, shape, jnp.float32) * s
    return {
        'x': nrm(ks[0], (BATCH, SEQ, d), 1.0),
        'c': nrm(ks[1], (BATCH, d), 1.0),
        'ctx': nrm(ks[2], (BATCH, CTX_LEN, d), 1.0),
        'c_ctx': nrm(ks[3], (d,), 1.0),
        'w_mod': nrm(ks[4], (DEPTH, d, 6 * d), 0.5 * d ** -0.5),
        'b_mod': nrm(ks[5], (DEPTH, 6 * d), 0.01),
        'hg_w_in': nrm(ks[6], (N_A_LAYERS, d, HG_IN_DIM), d ** -0.5),
        'hg_lb_logits': nrm(ks[7], (N_A_LAYERS, 2, HG_FDIM), 1.0),
        'hg_norm_w': 1.0 + nrm(ks[8], (N_A_LAYERS, HG_DV), 0.1),
        'hg_w_out': nrm(ks[9], (N_A_LAYERS, d, d), d ** -0.5),
        'sw_w_qkv': nrm(ks[10], (N_B_LAYERS, d, SW_Q_DIM + 2 * SW_KV_DIM), d ** -0.5),
        'sw_q_norm': 1.0 + nrm(ks[11], (N_B_LAYERS, SW_HEAD_DIM), 0.1),
        'sw_k_norm': 1.0 + nrm(ks[12], (N_B_LAYERS, SW_HEAD_DIM), 0.1),
        'sw_sink': nrm(ks[13], (N_B_LAYERS, SW_HEADS), 1.0),
        'sw_w_out': nrm(ks[14], (N_B_LAYERS, SW_Q_DIM, d), SW_Q_DIM ** -0.5),
        'ff_w_gate_up': nrm(ks[15], (N_DENSE_LAYERS, d, 2 * D_FF), d ** -0.5),
        'ff_w_down': nrm(ks[16], (N_DENSE_LAYERS, D_FF, d), D_FF ** -0.5),
        'moe_router': nrm(ks[17], (N_MOE_LAYERS, d, N_EXPERTS), d ** -0.5),
        'moe_w_gate_up': nrm(ks[18], (N_MOE_LAYERS, N_EXPERTS, d, 2 * D_EXPERT), d ** -0.5),
        'moe_w_down': nrm(ks[19], (N_MOE_LAYERS, N_EXPERTS, D_EXPERT, d), D_EXPERT ** -0.5),
    }


def reference(x, c, ctx, c_ctx, w_mod, b_mod, hg_w_in, hg_lb_logits, hg_norm_w, hg_w_out,
              sw_w_qkv, sw_q_norm, sw_k_norm, sw_sink, sw_w_out,
              ff_w_gate_up, ff_w_down, moe_router, moe_w_gate_up, moe_w_down):
    p_lb = jax.nn.softmax(hg_lb_logits.astype(jnp.float32), axis=0)
    lower_bounds = jnp.cumsum(p_lb, axis=0) - p_lb[:1]
    silu_c = jax.nn.silu(c)[:, None, :]
    silu_cc = jax.nn.silu(c_ctx)
    n_ctx = ctx.shape[1]
    for i in range(DEPTH):
        ctx_live = i < DEPTH - 1
        sh_a, sc_a, g_a, sh_f, sc_f, g_f = jnp.split(silu_c @ w_mod[i] + b_mod[i], 6, axis=-1)
        csh_a, csc_a, cg_a, csh_f, csc_f, cg_f = jnp.split(silu_cc @ w_mod[i] + b_mod[i], 6, axis=-1)
        h_lat = modulate(x, sh_a, sc_a)
        h_ctx = modulate(ctx, csh_a, csc_a)
        j = i // N_MIXERS
        if i % N_MIXERS == 0:
            y_lat, y_ctx = hgrn2_mixer(h_lat, h_ctx, hg_w_in[j], lower_bounds[j], hg_norm_w[j], hg_w_out[j], ctx_live)
        else:
            y_lat, y_ctx = swa_mixer(h_lat, h_ctx, sw_w_qkv[j], sw_q_norm[j], sw_k_norm[j], sw_sink[j], sw_w_out[j], ctx_live)
        x = x + g_a * y_lat
        h_lat = modulate(x, sh_f, sc_f)
        if ctx_live:
            ctx = ctx + cg_a * y_ctx
            h_tok = jnp.concatenate([modulate(ctx, csh_f, csc_f), h_lat], axis=1)
        else:
            h_tok = h_lat
        m = i // 2
        if i % 2 == 0:
            y = swiglu(h_tok, ff_w_gate_up[m], ff_w_down[m])
        else:
            y = moe_swiglu(h_tok, moe_router[m], moe_w_gate_up[m], moe_w_down[m])
        if ctx_live:
            ctx = ctx + cg_f * y[:, :n_ctx]
            x = x + g_f * y[:, n_ctx:]
        else:
            x = x + g_f * y
    return x
```

```python
import contextlib
import numpy as np
import concourse.bass as bass
import concourse.mybir as mybir
from concourse.bass_utils import run_bass_kernel_spmd

F32 = mybir.dt.float32
BF16 = mybir.dt.bfloat16
AF = mybir.ActivationFunctionType
ALU = mybir.AluOpType
AX = mybir.AxisListType


class V:
    __slots__ = ("b", "ap")

    def __init__(self, b, ap):
        self.b = b
        self.ap = ap


class DSem:
    def __init__(self, sem):
        self.sem = sem
        self.total = 0


class Buf:
    def __init__(self, name, h):
        self.name = name
        self.h = h
        self.last_w = None
        self.reads = []
        self.ds = None

    def __getitem__(self, idx):
        return V(self, self.h[idx])

    def v(self, ap):
        return V(self, ap)


class Op:
    __slots__ = ("eng", "seq", "emit", "waits", "signal", "dma", "sigval")


class Eng:
    def __init__(self, name):
        self.name = name
        self.ops = []
        self.sem = None
        self.seen = {}
        self.seen_dma = {}


class Prog:
    def __init__(self, nc):
        self.nc = nc
        self.es0 = contextlib.ExitStack()
        self.es = self.es0
        self.eng = {n: Eng(n) for n in ("pe", "dve", "act", "pool", "sp")}
        for n, e in self.eng.items():
            e.sem = self.es0.enter_context(nc.semaphore("s_" + n))
        self.dsems = []
        self.free_ds = []
        self.scope_bufs = [[]]
        self.uid = 0

    def sbuf(self, name, shape, dt):
        self.uid += 1
        h = self.es.enter_context(self.nc.sbuf_tensor("%s_%d" % (name, self.uid), list(shape), dt))
        b = Buf(name, h)
        self.scope_bufs[-1].append(b)
        return b

    def psum(self, name, shape, dt=F32):
        self.uid += 1
        h = self.es.enter_context(self.nc.psum_tensor("%s_%d" % (name, self.uid), list(shape), dt))
        b = Buf(name, h)
        self.scope_bufs[-1].append(b)
        return b

    def dram(self, name, shape, dt, kind="Internal"):
        t = self.nc.dram_tensor(name, list(shape), dt, kind=kind)
        return Buf(name, t.ap())

    def sub(self, name, ap):
        b = Buf(name, ap)
        self.scope_bufs[-1].append(b)
        return b

    @contextlib.contextmanager
    def scope(self):
        saved = self.es
        self.es = contextlib.ExitStack()
        self.scope_bufs.append([])
        try:
            yield
        finally:
            self.barrier()
            for b in self.scope_bufs.pop():
                if b.ds is not None:
                    self.free_ds.append(b.ds)
                    b.ds = None
            self.es.close()
            self.es = saved

    def _get_ds(self, b):
        if b.ds is None:
            if self.free_ds:
                b.ds = self.free_ds.pop()
            else:
                b.ds = DSem(self.es0.enter_context(self.nc.semaphore("d%d" % len(self.dsems))))
                self.dsems.append(b.ds)
        return b.ds

    def _need(self, E, tok, waits):
        if tok is None:
            return
        if tok[0] == "e":
            _, Pn, op = tok
            if E.seen.get(Pn.name, -1) >= op.seq:
                return
            E.seen[Pn.name] = op.seq
            op.signal = True
            waits.append(tok)
        else:
            _, ds, cnt = tok
            if E.seen_dma.get(id(ds), -1) >= cnt:
                return
            E.seen_dma[id(ds)] = cnt
            waits.append(tok)

    def _deps(self, E, reads, writes, pe_accum=False):
        waits = []
        for b in reads:
            self._need(E, b.last_w, waits)
        for b in writes:
            if not (pe_accum and b.last_w is not None and b.last_w[0] == "e" and b.last_w[1] is E):
                self._need(E, b.last_w, waits)
            for t in b.reads:
                self._need(E, t, waits)
        best = {}
        for t in waits:
            if t[0] == "e":
                k = ("e", t[1].name)
                if k not in best or best[k][2].seq < t[2].seq:
                    best[k] = t
            else:
                k = ("d", id(t[1]))
                if k not in best or best[k][2] < t[2]:
                    best[k] = t
        return list(best.values())

    def op(self, eng, emit, reads=(), writes=(), pe_accum=False):
        E = self.eng[eng]
        reads = list(dict.fromkeys(reads))
        writes = list(dict.fromkeys(writes))
        o = Op()
        o.eng = E
        o.seq = len(E.ops)
        o.emit = emit
        o.signal = False
        o.dma = None
        o.waits = self._deps(E, reads, writes, pe_accum)
        E.ops.append(o)
        tok = ("e", E, o)
        for b in reads:
            if b not in writes:
                b.reads.append(tok)
        for b in writes:
            b.last_w = tok
            b.reads = []
        return o

    def dma(self, q, out, in_, emit=None, inc=16, **kw):
        E = self.eng[q]
        dst, src = out.b, in_.b
        ds = self._get_ds(dst)
        o = Op()
        o.eng = E
        o.seq = len(E.ops)
        oa, ia = out.ap, in_.ap
        o.emit = emit if emit is not None else (lambda e: e.dma_start(out=oa, in_=ia, **kw))
        o.signal = False
        o.waits = self._deps(E, [src], [dst])
        ds.total += inc
        o.dma = (ds.sem, inc)
        E.ops.append(o)
        tok = ("d", ds, ds.total)
        src.reads.append(tok)
        dst.last_w = tok
        dst.reads = []
        return o

    def barrier(self):
        lasts = {}
        for n, E in self.eng.items():
            for oo in reversed(E.ops):
                if oo.emit is not None and oo.dma is None:
                    lasts[n] = ("e", E, oo)
                    break
        dtoks = [("d", ds, ds.total) for ds in self.dsems if ds.total > 0]
        for n, E in self.eng.items():
            waits = []
            for pn, t in lasts.items():
                if pn != n:
                    self._need(E, t, waits)
            for t in dtoks:
                self._need(E, t, waits)
            o = Op()
            o.eng = E
            o.seq = len(E.ops)
            o.emit = None
            o.signal = False
            o.dma = None
            o.waits = waits
            E.ops.append(o)

    @staticmethod
    def _bufs(*vs):
        return [v.b for v in vs if isinstance(v, V)]

    @staticmethod
    def _a(v):
        return v.ap if isinstance(v, V) else v

    def matmul(self, out, lhsT, rhs, start=True, stop=True):
        o, l, r = out.ap, lhsT.ap, rhs.ap
        return self.op("pe", lambda e: e.matmul(o, lhsT=l, rhs=r, start=start, stop=stop),
                       [lhsT.b, rhs.b], [out.b], pe_accum=True)

    def transpose(self, out, in_, ident):
        o, i, d = out.ap, in_.ap, ident.ap
        return self.op("pe", lambda e: e.transpose(out=o, in_=i, identity=d), [in_.b, ident.b], [out.b], pe_accum=True)

    def act(self, out, in_, func, bias=None, scale=None, accum_out=None):
        kw = {}
        rd = [in_.b]
        wr = [out.b]
        if bias is not None:
            kw["bias"] = self._a(bias)
            rd += self._bufs(bias)
        if scale is not None:
            kw["scale"] = self._a(scale)
            rd += self._bufs(scale)
        if accum_out is not None:
            kw["accum_out"] = accum_out.ap
            wr.append(accum_out.b)
        o, i = out.ap, in_.ap
        return self.op("act", lambda e: e.activation(out=o, in_=i, func=func, **kw), rd, wr)

    def tt(self, eng, out, a, b, op):
        o, x, y = out.ap, a.ap, b.ap
        return self.op(eng, lambda e: e.tensor_tensor(out=o, in0=x, in1=y, op=op), [a.b, b.b], [out.b])

    def ts(self, eng, out, a, s1, op0, s2=None, op1=None):
        o, x = out.ap, a.ap
        c1, c2 = self._a(s1), self._a(s2)
        kw = {}
        if op1 is not None:
            kw["op1"] = op1
        return self.op(eng, lambda e: e.tensor_scalar(out=o, in0=x, scalar1=c1, scalar2=c2, op0=op0, **kw),
                       [a.b] + self._bufs(s1, s2), [out.b])

    def stt(self, eng, out, a, scalar, b, op0, op1):
        o, x, y = out.ap, a.ap, b.ap
        c = self._a(scalar)
        return self.op(eng, lambda e: e.scalar_tensor_tensor(out=o, in0=x, scalar=c, in1=y, op0=op0, op1=op1),
                       [a.b, b.b] + self._bufs(scalar), [out.b])

    def copy(self, eng, out, in_):
        o, i = out.ap, in_.ap
        if eng == "act":
            return self.op("act", lambda e: e.copy(out=o, in_=i), [in_.b], [out.b])
        return self.op(eng, lambda e: e.tensor_copy(out=o, in_=i), [in_.b], [out.b])

    def memset(self, eng, out, val):
        o = out.ap
        return self.op(eng, lambda e: e.memset(o, val), [], [out.b])

    def reduce(self, eng, out, in_, op, axis=AX.X):
        o, i = out.ap, in_.ap
        return self.op(eng, lambda e: e.tensor_reduce(out=o, in_=i, axis=axis, op=op), [in_.b], [out.b])

    def scan(self, out, d0, d1, init, op0, op1):
        o, x, y = out.ap, d0.ap, d1.ap
        return self.op("dve", lambda e: e.tensor_tensor_scan(out=o, data0=x, data1=y, initial=init, op0=op0, op1=op1),
                       [d0.b, d1.b], [out.b])

    def recip(self, out, in_):
        o, i = out.ap, in_.ap
        return self.op("dve", lambda e: e.reciprocal(out=o, in_=i), [in_.b], [out.b])

    def finish(self):
        nc = self.nc
        Esp = self.eng["sp"]
        fin = Op()
        fin.eng = Esp
        fin.seq = len(Esp.ops)
        fin.emit = None
        fin.signal = False
        fin.dma = None
        fin.waits = [("d", ds, ds.total) for ds in self.dsems if ds.total > 0]
        Esp.ops.append(fin)
        for E in self.eng.values():
            v = 0
            for o in E.ops:
                if o.signal:
                    v += 1
                    o.sigval = v
        amap = {"pe": "tensor", "dve": "vector", "act": "scalar", "pool": "gpsimd", "sp": "sync"}
        self.stats = {n: (len(E.ops), sum(1 for o in E.ops if o.signal)) for n, E in self.eng.items()}
        with nc.Block() as block:
            for n, E in self.eng.items():
                def body(e, E=E):
                    for o in E.ops:
                        for t in o.waits:
                            if t[0] == "e":
                                e.wait_ge(t[1].sem, t[2].sigval)
                            else:
                                e.wait_ge(t[1].sem, t[2])
                        if o.emit is None:
                            continue
                        ins = o.emit(e)
                        if o.dma is not None:
                            ins.then_inc(o.dma[0], o.dma[1])
                        elif o.signal:
                            ins.then_inc(E.sem, 1)
                getattr(block, amap[n])(body)
        self.es0.close()


NT_LAT = 16
NT_CTX = 2
NTILE = NT_LAT + NT_CTX
NTOK = NTILE * 128
HG_H = 8


class Consts:
    pass


def make_consts(P, C):
    C.ident = P.sbuf("ident", [128, 128], F32)
    P.memset("pool", C.ident[:], 1.0)
    idh = C.ident.h
    P.op("pool", lambda e: e.affine_select(out=idh[:], in_=idh[:], pattern=[[-1, 128]], compare_op=ALU.is_equal,
                                           fill=0.0, base=0, channel_multiplier=1), [C.ident], [C.ident])
    C.identb = P.sbuf("identb", [128, 128], BF16)
    P.copy("pool", C.identb[:], C.ident[:])
    C.mask_f = P.sbuf("mask_f", [128, 128], F32)
    C.mask_b = P.sbuf("mask_b", [128, 128], F32)
    for m, mult in ((C.mask_f, -1), (C.mask_b, 1)):
        P.memset("pool", m[:], 1.0)
        mh = m.h
        cm = mult
        pat = [[-mult, 128]]
        P.op("pool", lambda e, mh=mh, cm=cm, pat=pat: e.affine_select(out=mh[:], in_=mh[:], pattern=pat, compare_op=ALU.is_ge,
                                                                         fill=0.0, base=0, channel_multiplier=cm), [m], [m])
    C.rst = P.sbuf("rst", [128, 512], F32)
    P.memset("pool", C.rst[:], 1.0)
    for j in range(4):
        P.memset("pool", C.rst[:, 128 * j:128 * j + 1], 0.0)
    C.zero1 = P.sbuf("zero1", [128, 1], F32)
    P.memset("pool", C.zero1[:], 0.0)


def hg_alloc_scan(P, C, G=2):
    W = C.W = Consts()
    W.G = G
    n = G * 128
    W.q = [P.sbuf("hq%d" % i, [128, HG_H, n], BF16) for i in range(2)]
    W.k = [P.sbuf("hk%d" % i, [128, HG_H, n], BF16) for i in range(2)]
    W.lf = [P.sbuf("hlf%d" % i, [128, HG_H, n], F32) for i in range(2)]
    W.v = [P.sbuf("hv%d" % i, [128, G, 1024], BF16) for i in range(2)]
    W.A = [P.sbuf("hA%d" % i, [128, n], F32) for i in range(2)]
    W.Cum = [P.sbuf("hCum%d" % i, [128, n], F32) for i in range(2)]
    W.R = [[P.sbuf("hR%d_%d" % (dd, i), [128, G, 4], F32) for i in range(2)] for dd in range(2)]
    W.argq = [P.sbuf("hargq%d" % i, [128, n], F32) for i in range(2)]
    W.argK = [[P.sbuf("hargK%d" % dd, [128, G, 4, 128], F32)] * 2 for dd in range(2)]
    W.EK = [P.sbuf("hEK%d" % i, [128, G, 4, 128], F32) for i in range(2)]
    W.args = [P.sbuf("hargs%d" % i, [128, n], F32) for i in range(2)]
    W.arge = [P.sbuf("harge%d" % i, [128, n], F32) for i in range(2)]
    for dd in range(2):
        P.memset("pool", W.argK[dd][0][:], 0.0)
        for i in range(2):
            P.memset("pool", W.R[dd][i][:], 0.0)
    W.qd = [P.sbuf("hqd%d" % h, [128, n], BF16) for h in range(HG_H)]
    W.qs = [P.sbuf("hqs%d" % h, [128, n], BF16) for h in range(HG_H)]
    W.ke = [P.sbuf("hke%d" % h, [128, n], BF16) for h in range(HG_H)]
    W.K = [P.sbuf("hK%d" % h, [128, G, 4, 128], BF16) for h in range(HG_H)]
    W.dec = [P.sbuf("hdec%d" % h, [128, G], F32) for h in range(HG_H)]
    W.scm = [P.sbuf("hscm%d" % i, [128, 128], BF16) for i in range(2)]
    W.keT = [P.sbuf("hkeT%d" % i, [128, 128], BF16) for i in range(2)]
    W.ps_sc = [P.psum("ps_sc%d" % i, [128, 128], F32) for i in range(2)]
    W.ps_kt = [P.psum("ps_kt%d" % i, [128, 128], BF16) for i in range(2)]
    W.ps_o = [P.psum("ps_o%d" % i, [128, 128], F32) for i in range(2)]
    W.ps_ds = [P.psum("ps_ds%d" % i, [128, 128], F32) for i in range(2)]
    W.cnt = 0
    W.gcnt = 0


def hg_prep_head(P, C, d, h, gi, nt, lbq):
    W = C.W
    n = nt * 128
    i = W.cnt % 2
    W.cnt += 1
    lf = W.lf[gi][:, h, 0:n]
    kt = W.k[gi][:, h, 0:n]
    qt = W.q[gi][:, h, 0:n]
    A, Cum, R, argq, argK, args, arge, EK = W.A[i], W.Cum[i], W.R[d][i], W.argq[i], W.argK[d][i], W.args[i], W.arge[i], W.EK[i]
    A3 = A.h[:, 0:n].rearrange("p (g t) -> p g t", t=128)
    P.scan(A[:, 0:n], C.rst[:, 0:n], lf, 0.0, ALU.mult, ALU.add)
    tot_bc = A.v(A3[:, :, 127:128].to_broadcast([128, nt, 128]))
    if d == 0:
        cum = A
    else:
        cum = Cum
        P.tt("dve", Cum[:, 0:n], lf, A[:, 0:n], ALU.subtract)
        c3 = Cum.h[:, 0:n].rearrange("p (g t) -> p g t", t=128)
        P.tt("dve", Cum.v(c3), Cum.v(c3), tot_bc, ALU.add)
    c3 = cum.h[:, 0:n].rearrange("p (g t) -> p g t", t=128)
    c4 = cum.h[:, 0:n].rearrange("p (g i t) -> p g i t", i=4, t=32)
    if d == 0:
        P.copy("dve", R[:, 0:nt, 1:4], cum.v(c4[:, :, 0:3, 31]))
    else:
        P.copy("dve", R[:, 0:nt, 0:3], cum.v(c4[:, :, 1:4, 0]))
    aq4 = argq.h[:, 0:n].rearrange("p (g i t) -> p g i t", i=4, t=32)
    P.tt("dve", argq.v(aq4), cum.v(c4), R.v(R.h[:, 0:nt, :].unsqueeze(3).to_broadcast([128, nt, 4, 32])), ALU.subtract)
    P.act(argq[:, 0:n], argq[:, 0:n], AF.Exp)
    P.stt("dve", W.qd[h][:, 0:n], qt, lbq, argq[:, 0:n], ALU.mult, ALU.mult)
    for bi in range(4):
        rng = slice(0, 32 * (bi + 1)) if d == 0 else slice(32 * bi, 128)
        ln = rng.stop - rng.start
        P.tt("dve", argK[:, 0:nt, bi, rng], R.v(R.h[:, 0:nt, bi:bi + 1].to_broadcast([128, nt, ln])), cum.v(c3[:, :, rng]), ALU.subtract)
    P.act(EK[:, 0:nt], argK[:, 0:nt], AF.Exp)
    kt3 = W.k[gi].h[:, h, 0:n].rearrange("p (g t) -> p g t", t=128)
    P.tt("dve", W.K[h][:, 0:nt], EK[:, 0:nt], W.k[gi].v(kt3.unsqueeze(2).to_broadcast([128, nt, 4, 128])), ALU.mult)
    P.act(args[:, 0:n], cum[:, 0:n], AF.Exp)
    P.stt("dve", W.qs[h][:, 0:n], qt, lbq, args[:, 0:n], ALU.mult, ALU.mult)
    ae3 = arge.h[:, 0:n].rearrange("p (g t) -> p g t", t=128)
    P.tt("dve", arge.v(ae3), tot_bc, cum.v(c3), ALU.subtract)
    P.act(arge[:, 0:n], arge[:, 0:n], AF.Exp)
    P.tt("dve", W.ke[h][:, 0:n], arge[:, 0:n], kt, ALU.mult)
    P.act(W.dec[h][:, 0:nt], A.v(A3[:, :, 127]), AF.Exp)


def hg_step(P, C, d, h, gi, j, S, Sb, o_out):
    W = C.W
    i = W.gcnt % 2
    W.gcnt += 1
    sl = slice(128 * j, 128 * j + 128)
    mask = C.mask_f if d == 0 else C.mask_b
    ps_sc, ps_kt, ps_o, ps_ds = W.ps_sc[i], W.ps_kt[i], W.ps_o[i], W.ps_ds[i]
    for bi in range(4):
        P.matmul(ps_sc[:, 32 * bi:32 * bi + 32], W.K[h][:, j, bi, :], W.qd[h][:, 128 * j + 32 * bi:128 * j + 32 * bi + 32])
    P.tt("dve", W.scm[i][:], ps_sc[:], mask[:], ALU.mult)
    P.transpose(ps_kt[:], W.ke[h][:, sl], C.identb[:])
    P.copy("act", W.keT[i][:], ps_kt[:])
    vv = W.v[gi][:, j, 128 * h:128 * h + 128]
    P.matmul(ps_o[:], W.scm[i][:], vv, start=True, stop=False)
    P.matmul(ps_o[:], W.qs[h][:, sl], Sb[h][:], start=False, stop=True)
    P.matmul(ps_ds[:], W.keT[i][:], vv)
    P.copy("act", o_out, ps_o[:])
    P.stt("dve", S[h][:], S[h][:], W.dec[h][:, j:j + 1], ps_ds[:], ALU.mult, ALU.add)
    P.copy("pool", Sb[h][:], S[h][:])


def hg_scan_segment(P, C, d, tiles, S, Sb, D, lbq, o_tile_cb):
    W = C.W
    G = W.G
    groups = [tiles[a:a + G] for a in range(0, len(tiles), G)]
    for gidx, grp in enumerate(groups):
        gi = gidx % 2
        nt = len(grp)
        t0 = min(grp) * 128
        n = nt * 128
        P.dma("sp", W.q[gi][:, :, 0:n], D["qS"].v(D["qS"].h[:, :, t0:t0 + n].rearrange("h p t -> p h t")))
        P.dma("sp", W.k[gi][:, :, 0:n], D["ktS"].v(D["ktS"].h[d, :, :, t0:t0 + n].rearrange("h p t -> p h t")))
        P.dma("sp", W.lf[gi][:, :, 0:n], D["lfS"].v(D["lfS"].h[d, :, :, t0:t0 + n].rearrange("h p t -> p h t")))
        P.dma("sp", W.v[gi][:, 0:nt, :], D["vS"].v(D["vS"].h[t0:t0 + n, :].rearrange("(g p) f -> p g f", p=128)))
        for h in range(HG_H):
            hg_prep_head(P, C, d, h, gi, nt, lbq[d][h])
        for tile in grp:
            j = tile - min(grp)
            ob = o_tile_cb(tile, None)
            for h in range(HG_H):
                hg_step(P, C, d, h, gi, j, S, Sb, ob[:, 128 * h:128 * h + 128])
            o_tile_cb(tile, ob)


D_MODEL = 1024
EPS = 1e-6
D_FF = 2816
D_EXPERT = 3584
N_EXPERTS = 8


def rr(ap, s, **kw):
    return ap.rearrange(s, **kw)


def stage_mod(P, C, I):
    C.modcol = P.sbuf("modcol", [128, 4, 48, 2], F32)
    C.modD = P.dram("modD", [4, 2, 6144], F32)
    with P.scope():
        cT = P.sbuf("cT", [128, 8, 2], F32)
        for r_ in range(2):
            P.dma("sp", cT[:, :, r_], I["cvec"].v(rr(I["cvec"].h[r_], "(c p) -> p c", p=128)), allow_slow_non_contiguous=True)
        sT = P.sbuf("sT", [128, 8, 2], F32)
        P.act(sT[:], cT[:], AF.Silu)
        wbuf = [P.sbuf("wm%d" % i, [128, 8, 512], F32) for i in range(2)]
        bb = [P.sbuf("bm%d" % i, [2, 512], F32) for i in range(2)]
        row = [P.sbuf("mrow%d" % i, [2, 6144], F32) for i in range(2)]
        ps = [P.psum("psm%d" % i, [2, 512], F32) for i in range(2)]
        pst = [P.psum("pst%d" % i, [128, 96], F32) for i in range(2)]
        k = 0
        for l in range(4):
            rw = row[l % 2]
            for fb in range(12):
                w = wbuf[k % 2]
                b = bb[k % 2]
                p = ps[k % 2]
                k += 1
                fs = slice(fb * 512, fb * 512 + 512)
                P.dma("sp", w[:], I["w_mod"].v(rr(I["w_mod"].h[l], "(c p) f -> p c f", p=128)[:, :, fs]))
                P.dma("sp", b[:], I["b_mod"].v(I["b_mod"].h[l, fs].partition_broadcast(2)))
                for dc in range(8):
                    P.matmul(p[:], sT[:, dc, :], w[:, dc, :], start=(dc == 0), stop=(dc == 7))
                P.tt("dve", rw[:, fs], p[:], b[:], ALU.add)
            P.dma("sp", C.modD.v(C.modD.h[l]), rw[:])
            pt = pst[l % 2]
            for ch in range(48):
                P.transpose(pt[:, 2 * ch:2 * ch + 2], rw[:, 128 * ch:128 * ch + 128], C.ident[0:2, 0:2])
            P.copy("dve", C.modcol.v(rr(C.modcol.h[:, l], "p c r -> p (c r)")), pt[:])
        for c0 in (8, 32):
            P.ts("dve", C.modcol[:, :, c0:c0 + 8, :], C.modcol[:, :, c0:c0 + 8, :], 1.0, ALU.add)


def stage_lb(P, C, I):
    C.lbcol = P.sbuf("lbcol", [128, 2, 8, 2], F32)
    C.onecol = P.sbuf("onecol", [128, 2], F32)
    P.memset("pool", C.onecol[:, 0:1], 1.0)
    P.memset("pool", C.onecol[:, 1:2], -1.0)
    with P.scope():
        l0 = P.sbuf("lb0", [2, 1024], F32)
        l1 = P.sbuf("lb1", [2, 1024], F32)
        P.dma("sp", l0[:], I["hg_lb"].v(I["hg_lb"].h[0]))
        P.dma("sp", l1[:], I["hg_lb"].v(I["hg_lb"].h[1]))
        P.tt("dve", l1[:], l1[:], l0[:], ALU.subtract)
        P.act(l1[:], l1[:], AF.Sigmoid, scale=-1.0)
        pt = P.psum("pslb", [128, 16], F32)
        for h in range(8):
            P.transpose(pt[:, 2 * h:2 * h + 2], l1[:, 128 * h:128 * h + 128], C.ident[0:2, 0:2])
        P.copy("dve", C.lbcol.v(rr(C.lbcol.h[:, 0], "p h d -> p (h d)")), pt[:])
        P.ts("dve", C.lbcol[:, 1], C.lbcol[:, 0], -1.0, ALU.mult)
    C.lbq = [[[C.onecol[:, 0:1] for h in range(8)] for d in range(2)],
             [[C.lbcol[:, 0, h, d:d + 1] for h in range(8)] for d in range(2)]]
    C.nlbq = [[[C.onecol[:, 1:2] for h in range(8)] for d in range(2)],
              [[C.lbcol[:, 1, h, d:d + 1] for h in range(8)] for d in range(2)]]


class Modulator:
    def __init__(self, P, C, l, sub, router=None, nbuf=2):
        self.P, self.C, self.l, self.router = P, C, l, router
        self.nbuf = nbuf
        self.sh0, self.sc0 = (0, 8) if sub == 0 else (24, 32)
        self.junk = P.sbuf("mjunk", [128, 1024], BF16)
        self.xn = [P.sbuf("mxn%d" % i, [128, 1024], F32) for i in range(2)]
        self.st = [P.sbuf("mst%d" % i, [128, 4], F32) for i in range(2)]
        self.pt = [P.psum("mpt%d" % i, [128, 1024], F32) for i in range(nbuf)]
        if router is not None:
            self.h32 = [P.sbuf("mh32%d" % i, [128, 8, 128], F32) for i in range(2)]
            self.plg = [P.psum("mplg%d" % i, [128, 8], F32) for i in range(2)]
            self.plgT = [P.psum("mplgT%d" % i, [8, 128], F32) for i in range(2)]
            self.lgT = [P.sbuf("mlgT%d" % i, [8, 128], F32) for i in range(2)]
            self.rt = [P.sbuf("mrt%d" % i, [128, 40], F32) for i in range(2)]
        self.n = 0

    def tile(self, t, out):
        P, C, l = self.P, self.C, self.l
        i = self.n % 2
        self.n += 1
        r = 1 if t < NT_CTX else 0
        x = C.X[t]
        s = self.st[i]
        xn, pt = self.xn[i], self.pt[i % self.nbuf]
        P.act(self.junk[:], x[:], AF.Square, accum_out=s[:, 0:1])
        P.act(s[:, 1:2], s[:, 0:1], AF.Ln, bias=C.epscol[:, 0:1], scale=1.0 / D_MODEL)
        P.act(s[:, 2:3], s[:, 1:2], AF.Exp, scale=-0.5)
        P.ts("dve", xn[:], x[:], s[:, 2:3], ALU.mult)
        for dc in range(8):
            P.transpose(pt[:, 128 * dc:128 * dc + 128], xn[:, 128 * dc:128 * dc + 128], C.ident[:])
        for dc in range(8):
            o = out(dc)
            sc = C.modcol[:, l, self.sc0 + dc, r:r + 1]
            sh = C.modcol[:, l, self.sh0 + dc, r:r + 1]
            o1 = o if self.router is None else self.h32[i][:, dc, :]
            if dc % 2 == 0:
                P.act(o1, pt[:, 128 * dc:128 * dc + 128], AF.Identity, bias=sh, scale=sc)
            else:
                P.ts("dve", o1, pt[:, 128 * dc:128 * dc + 128], sc, ALU.mult, sh, ALU.add)
            if self.router is not None:
                P.copy("pool", o, self.h32[i][:, dc, :])
        if self.router is not None:
            for dc in range(8):
                P.matmul(self.plgT[i][:], self.router["w"][:, dc, :], self.h32[i][:, dc, :], start=(dc == 0), stop=(dc == 7))
            P.copy("dve", self.lgT[i][:], self.plgT[i][:])
            P.transpose(self.plg[i][:], self.lgT[i][:], C.ident[0:8, 0:8])
            top2_gates(P, self.plg[i], self.rt[i], self.router["gates"][:, t, :])


def stage_modulate(P, C, l, sub, tiles, hT, router=None):
    with P.scope():
        M = Modulator(P, C, l, sub, router)
        for t in tiles:
            M.tile(t, lambda dc, t=t: hT[:, dc, 128 * t:128 * t + 128])


def top2_gates(P, plg, rt, gout):
    lg = rt[:, 0:8]
    P.copy("dve", lg, plg[:])
    m1 = rt[:, 8:9]
    P.reduce("dve", m1, lg, ALU.max)
    mk1 = rt[:, 16:24]
    P.ts("dve", mk1, lg, m1, ALU.is_ge)
    l2 = rt[:, 24:32]
    P.stt("dve", l2, mk1, -1e30, lg, ALU.mult, ALU.add)
    m2 = rt[:, 9:10]
    P.reduce("dve", m2, l2, ALU.max)
    mk2 = rt[:, 32:40]
    P.ts("dve", mk2, l2, m2, ALU.is_ge)
    dlt = rt[:, 10:11]
    P.tt("dve", dlt, m1, m2, ALU.subtract)
    P.act(rt[:, 11:12], dlt, AF.Sigmoid)
    P.act(rt[:, 12:13], dlt, AF.Sigmoid, scale=-1.0)
    P.ts("dve", mk1, mk1, rt[:, 11:12], ALU.mult)
    P.stt("dve", gout, mk2, rt[:, 12:13], mk1, ALU.mult, ALU.add)


def stage_hg_proj(P, C, j, hT, I, D):
    win = rr(I["hg_w_in"].h[j], "(c p) f -> p c f", p=128)
    with P.scope():
        wb = [P.sbuf("hgw%d" % i, [128, 8, 1024], BF16) for i in range(2)]
        ps = [P.psum("hgps%d" % i, [128, 512], F32) for i in range(4)]
        ob16 = [P.sbuf("hgo16_%d" % i, [128, 512], BF16) for i in range(3)]
        sn = [P.sbuf("hgsn%d" % i, [128, 512], F32) for i in range(2)]
        lfb = [P.sbuf("hglf%d" % i, [128, 512], F32) for i in range(2)]
        tmo = [P.sbuf("hgtm%d" % i, [128, 1024], BF16) for i in range(2)]
        sts = [(a, min(a + 512, NTOK)) for a in range(0, NTOK, 512)]
        k = 0
        k16 = 0
        kt = 0
        for gi_, g in enumerate((0, 2, 3, 1, 4)):
            w = wb[gi_ % 2]
            P.dma("pool", w[:], I["hg_w_in"].v(win[:, :, 1024 * g:1024 * g + 1024]))
            if g in (0, 2, 3):
                for h in range(8):
                    for (a, b) in sts:
                        n = b - a
                        p = ps[k % 4]
                        k += 1
                        for dc in range(8):
                            P.matmul(p[:, 0:n], w[:, dc, 128 * h:128 * h + 128], hT[:, dc, a:b], start=(dc == 0), stop=(dc == 7))
                        o = ob16[k16 % 3]
                        k16 += 1
                        if g == 0:
                            P.act(o[:, 0:n], p[:, 0:n], AF.Silu)
                            P.dma("sp", D["qS"].v(D["qS"].h[h, :, a:b]), o[:, 0:n])
                        else:
                            d = g - 2
                            s = sn[kt % 2]
                            lfo = lfb[kt % 2]
                            kt += 1
                            P.act(s[:, 0:n], p[:, 0:n], AF.Sigmoid, scale=-1.0)
                            P.act(lfo[:, 0:n], s[:, 0:n], AF.Ln, bias=C.onecol[:, 0:1], scale=C.nlbq[j][d][h])
                            P.copy("pool", o[:, 0:n], s[:, 0:n])
                            P.dma("sp", D["lfS"].v(D["lfS"].h[d, h, :, a:b]), lfo[:, 0:n])
                            P.dma("sp", D["ktS"].v(D["ktS"].h[d, h, :, a:b]), o[:, 0:n])
            else:
                dst = D["vS"] if g == 1 else D["gS"]
                for t in range(NTILE):
                    ot = tmo[t % 2]
                    for hf in range(2):
                        p = ps[k % 4]
                        k += 1
                        for dc in range(8):
                            P.matmul(p[:], hT[:, dc, 128 * t:128 * t + 128], w[:, dc, 512 * hf:512 * hf + 512], start=(dc == 0), stop=(dc == 7))
                        if g == 1:
                            P.copy("act", ot[:, 512 * hf:512 * hf + 512], p[:])
                        else:
                            P.act(ot[:, 512 * hf:512 * hf + 512], p[:], AF.Silu)
                    P.dma("sp", dst.v(dst.h[128 * t:128 * t + 128, :]), ot[:])


GROUPS = [[0, 1], [2, 3], [4, 5], [6, 7]]


def stage_hg_scan(P, C, j, I, D):
    with P.scope():
        hg_alloc_scan(P, C, G=2)
        S = [P.sbuf("S%d" % h, [128, 128], F32) for h in range(8)]
        Sb = [P.sbuf("Sb%d" % h, [128, 128], BF16) for h in range(8)]
        obuf = [P.sbuf("hob%d" % i, [128, 1024], F32) for i in range(2)]
        oprev = [P.sbuf("hop%d" % i, [128, 1024], F32) for i in range(2)]
        oS = D["oS"]
        cnt = [0]

        def zero_state():
            for h in range(8):
                P.memset("pool", S[h][:], 0.0)
                P.memset("pool", Sb[h][:], 0.0)

        def cb_first(tile, ob):
            if ob is None:
                cnt[0] += 1
                return obuf[cnt[0] % 2]
            P.dma("sp", oS.v(oS.h[128 * tile:128 * tile + 128, :]), ob[:])

        def cb_second(tile, ob):
            if ob is None:
                cnt[0] += 1
                op = oprev[cnt[0] % 2]
                P.dma("sp", op[:], oS.v(oS.h[128 * tile:128 * tile + 128, :]))
                return obuf[cnt[0] % 2]
            op = oprev[cnt[0] % 2]
            P.tt("pool", op[:], op[:], ob[:], ALU.add)
            P.dma("sp", oS.v(oS.h[128 * tile:128 * tile + 128, :]), op[:])

        lbq = C.lbq[j]
        zero_state()
        hg_scan_segment(P, C, 1, [1, 0], S, Sb, D, lbq, cb_first)
        zero_state()
        hg_scan_segment(P, C, 0, [0, 1], S, Sb, D, lbq, cb_second)
        hg_scan_segment(P, C, 0, list(range(2, NTILE)), S, Sb, D, lbq, cb_first)
        snd, rcv = D["snd"], D["rcv"]
        for h in range(8):
            P.dma("sp", snd.v(snd.h[128 * h:128 * h + 128, :]), S[h][:])
        sh, rh = snd.h, rcv.h
        P.dma("pool", rcv.v(rh), snd.v(sh), inc=1,
              emit=lambda e: e.collective_compute("AllGather", ALU.bypass, replica_groups=GROUPS, ins=[sh.opt()], outs=[rh.opt()]))
        r0, r1 = obuf[0], obuf[1]
        P.dma("sp", r0.v(rr(r0.h[:], "p (h v) -> p h v", v=128)), rcv.v(rr(rcv.h[0:1024, :], "(h p) v -> p h v", p=128)))
        P.dma("sp", r1.v(rr(r1.h[:], "p (h v) -> p h v", v=128)), rcv.v(rr(rcv.h[1024:2048, :], "(h p) v -> p h v", p=128)))
        for h in range(8):
            P.ts("dve", S[h][:], r0[:, 128 * h:128 * h + 128], C.sel[:, 0:1], ALU.mult)
            P.stt("dve", S[h][:], r1[:, 128 * h:128 * h + 128], C.sel[:, 1:2], S[h][:], ALU.mult, ALU.add)
            P.copy("pool", Sb[h][:], S[h][:])
        hg_scan_segment(P, C, 1, list(range(NTILE - 1, 1, -1)), S, Sb, D, lbq, cb_second)


def load_gbc(P, C, l, which, r, name):
    g = P.sbuf(name, [128, 1024], F32)
    c0 = 2048 if which == 0 else 5120
    P.dma("sp", g[:], C.modD.v(C.modD.h[l, r, c0:c0 + 1024].partition_broadcast(128)))
    return g


def stage_hg_readout(P, C, l, j, I, D):
    with P.scope():
        wo = P.sbuf("hwo", [128, 8, 1024], BF16)
        P.dma("pool", wo[:], I["hg_w_out"].v(rr(I["hg_w_out"].h[j], "(c p) f -> p c f", p=128)))
        nw = P.sbuf("hnw", [128, 128], F32)
        P.dma("sp", nw[:], I["hg_norm_w"].v(I["hg_norm_w"].h[j].partition_broadcast(128)))
        gb = [load_gbc(P, C, l, 0, 0, "hgb0"), load_gbc(P, C, l, 0, 1, "hgb1")]
        ob = [P.sbuf("ro%d" % i, [128, 1024], F32) for i in range(2)]
        sg = [P.sbuf("rsg%d" % i, [128, 1024], BF16) for i in range(2)]
        junk = P.sbuf("rjunk", [128, 128], F32)
        st = [P.sbuf("rst%d" % i, [128, 16], F32) for i in range(2)]
        aT = [P.sbuf("raT%d" % i, [128, 8, 128], BF16) for i in range(2)]
        tmp = [P.sbuf("rtmp%d" % i, [128, 512], F32) for i in range(2)]
        pt = [P.psum("rpt%d" % i, [128, 1024], F32) for i in range(2)]
        py = [P.psum("rpy%d" % i, [128, 512], F32) for i in range(2)]
        ky = 0
        for t in range(NTILE):
            i = t % 2
            r = 1 if t < NT_CTX else 0
            o = ob[i]
            P.dma("sp", o[:], D["oS"].v(D["oS"].h[128 * t:128 * t + 128, :]))
            P.dma("sp", sg[i][:], D["gS"].v(D["gS"].h[128 * t:128 * t + 128, :]))
            s = st[i]
            for h in range(8):
                P.act(junk[:], o[:, 128 * h:128 * h + 128], AF.Square, accum_out=s[:, h:h + 1])
            P.act(s[:, 8:16], s[:, 0:8], AF.Ln, bias=C.epscol[:, 0:1], scale=1.0 / 128)
            P.act(s[:, 8:16], s[:, 8:16], AF.Exp, scale=-0.5)
            o3 = rr(o.h[:], "p (h v) -> p h v", v=128)
            P.tt("dve", o.v(o3), o.v(o3), s.v(s.h[:, 8:16].unsqueeze(2).to_broadcast([128, 8, 128])), ALU.mult)
            P.tt("pool", o.v(o3), o.v(o3), nw.v(nw.h[:].unsqueeze(1).to_broadcast([128, 8, 128])), ALU.mult)
            P.tt("dve", o[:], o[:], sg[i][:], ALU.mult)
            for dc in range(8):
                P.transpose(pt[i][:, 128 * dc:128 * dc + 128], o[:, 128 * dc:128 * dc + 128], C.ident[:])
            P.copy("act", aT[i].v(rr(aT[i].h[:, 0:4, :], "p c t -> p (c t)")), pt[i][:, 0:512])
            P.copy("act", aT[i].v(rr(aT[i].h[:, 4:8, :], "p c t -> p (c t)")), pt[i][:, 512:1024])
            for hf in range(2):
                p = py[ky % 2]
                tm = tmp[ky % 2]
                ky += 1
                for dc in range(8):
                    P.matmul(p[:], aT[i][:, dc, :], wo[:, dc, 512 * hf:512 * hf + 512], start=(dc == 0), stop=(dc == 7))
                P.tt("dve", tm[:], p[:], gb[r][:, 512 * hf:512 * hf + 512], ALU.mult)
                P.tt("pool", C.X[t][:, 512 * hf:512 * hf + 512], C.X[t][:, 512 * hf:512 * hf + 512], tm[:], ALU.add)


def stage_ffn(P, C, l, hT, tiles, specs, gates=None):
    with P.scope():
        gb = [load_gbc(P, C, l, 1, 0, "fgb0"), load_gbc(P, C, l, 1, 1, "fgb1")]
        wg = [P.sbuf("fwg%d" % i, [128, 8, 512], BF16) for i in range(2)]
        wu = [P.sbuf("fwu%d" % i, [128, 8, 512], BF16) for i in range(2)]
        wd = [P.sbuf("fwd%d" % i, [128, 4, 1024], BF16) for i in range(2)]
        hd = [P.sbuf("fhd%d" % i, [128, 4, 512], BF16) for i in range(2)]
        sgl = [P.sbuf("fsg%d" % i, [128, 512], F32) for i in range(2)]
        tmp = [P.sbuf("ftmp%d" % i, [128, 512], F32) for i in range(2)]
        pg = [P.psum("fpg%d" % i, [128, 512], F32) for i in range(2)]
        pu = [P.psum("fpu%d" % i, [128, 512], F32) for i in range(2)]
        py = [P.psum("fpy%d" % i, [128, 512], F32) for i in range(2)]
        t0 = min(tiles) * 128
        t1 = (max(tiles) + 1) * 128
        sts = [(a, min(a + 512, t1)) for a in range(t0, t1, 512)]
        kw = 0
        kp = 0
        ks = 0
        ky = 0
        for (gu_buf, gu_ap, d_buf, d_ap, F, e) in specs:
            gu3 = rr(gu_ap, "(c p) f -> p c f", p=128)
            for f0 in range(0, F, 512):
                fn = min(512, F - f0)
                nfc = fn // 128
                i = kw % 2
                kw += 1
                P.dma("pool", wg[i][:, :, 0:fn], gu_buf.v(gu3[:, :, f0:f0 + fn]))
                P.dma("pool", wu[i][:, :, 0:fn], gu_buf.v(gu3[:, :, F + f0:F + f0 + fn]))
                P.dma("pool", wd[i][:, 0:nfc, :], d_buf.v(rr(d_ap[f0:f0 + fn, :], "(c p) f -> p c f", p=128)))
                for (a, b) in sts:
                    n = b - a
                    h = hd[ks % 2]
                    ks += 1
                    for fc in range(nfc):
                        g_ps, u_ps = pg[kp % 2], pu[kp % 2]
                        s = sgl[kp % 2]
                        kp += 1
                        for dc in range(8):
                            P.matmul(g_ps[:, 0:n], wg[i][:, dc, 128 * fc:128 * fc + 128], hT[:, dc, a:b], start=(dc == 0), stop=(dc == 7))
                        for dc in range(8):
                            P.matmul(u_ps[:, 0:n], wu[i][:, dc, 128 * fc:128 * fc + 128], hT[:, dc, a:b], start=(dc == 0), stop=(dc == 7))
                        P.act(s[:, 0:n], g_ps[:, 0:n], AF.Silu)
                        P.tt("dve", h[:, fc, 0:n], s[:, 0:n], u_ps[:, 0:n], ALU.mult)
                    for t in range(a // 128, b // 128):
                        r = 1 if t < NT_CTX else 0
                        for hf in range(2):
                            p = py[ky % 2]
                            tm = tmp[ky % 2]
                            ky += 1
                            for fc in range(nfc):
                                P.matmul(p[:], h[:, fc, 128 * t - a:128 * t - a + 128], wd[i][:, fc, 512 * hf:512 * hf + 512],
                                         start=(fc == 0), stop=(fc == nfc - 1))
                            gsl = gb[r][:, 512 * hf:512 * hf + 512]
                            if e is None:
                                P.tt("dve", tm[:], p[:], gsl, ALU.mult)
                            else:
                                P.stt("dve", tm[:], p[:], gates[:, t, e:e + 1], gsl, ALU.mult, ALU.mult)
                            xs = C.X[t][:, 512 * hf:512 * hf + 512]
                            P.tt("pool", xs, xs, tm[:], ALU.add)


SW_H = 16
SW_KV = 4
HALO = NTILE
NKT = NTILE + 1


def swa_normrope(P, C, W, src_ps, nh, nw, rope, out_bf, i):
    n = nh * 64
    xf = W.xf[i]
    sq = W.sq[i]
    st = W.nst[i]
    P.copy("act", xf[:, 0:n], src_ps)
    P.tt("pool", sq[:, 0:n], xf[:, 0:n], xf[:, 0:n], ALU.mult)
    P.reduce("dve", st[:, 0:nh], sq.v(rr(sq.h[:, 0:n], "p (h d) -> p h d", d=64)), ALU.add)
    P.act(st[:, 16:16 + nh], st[:, 0:nh], AF.Ln, bias=C.epscol[:, 0:1], scale=1.0 / 64)
    P.act(st[:, 16:16 + nh], st[:, 16:16 + nh], AF.Exp, scale=-0.5)
    x3 = rr(xf.h[:, 0:n], "p (h d) -> p h d", d=64)
    P.tt("dve", xf.v(x3), xf.v(x3), st.v(st.h[:, 16:16 + nh].unsqueeze(2).to_broadcast([128, nh, 64])), ALU.mult)
    if rope is None:
        P.tt("dve", out_bf.b.v(rr(out_bf.ap, "p (h d) -> p h d", d=64)), xf.v(x3), nw.v(nw.h[:].unsqueeze(1).to_broadcast([128, nh, 64])), ALU.mult)
        return
    P.tt("pool", xf.v(x3), xf.v(x3), nw.v(nw.h[:].unsqueeze(1).to_broadcast([128, nh, 64])), ALU.mult)
    t1 = W.t1[i]
    t13 = rr(t1.h[:, 0:n], "p (h d) -> p h d", d=64)
    P.tt("dve", t1.v(t13), xf.v(x3), rope.b.v(rope.ap[:, 0:64].unsqueeze(1).to_broadcast([128, nh, 64])), ALU.mult)
    x5 = rr(xf.h[:, 0:n], "p (h a b c) -> p h a b c", a=2, b=2, c=16)
    s5 = rr(sq.h[:, 0:n], "p (h a b c) -> p h a b c", a=2, b=2, c=16)
    ss4 = rr(rope.ap[:, 64:128], "p (a b c) -> p a b c", a=2, b=2, c=16)
    for part in range(2):
        P.tt("pool", sq.v(s5[:, :, :, part, :]), xf.v(x5[:, :, :, 1 - part, :]),
             rope.b.v(ss4[:, :, part, :].unsqueeze(1).to_broadcast([128, nh, 2, 16])), ALU.mult)
    P.tt("dve", out_bf, t1[:, 0:n], sq[:, 0:n], ALU.add)


def stage_swa(P, C, l, j, I, D, ctx_live):
    kTs, V2s = D["kTs"], D["V2s"]
    wq3 = rr(I["sw_w_qkv"].h[j], "(c p) f -> p c f", p=128)
    with P.scope():
        W = Consts()
        W.xf = [P.sbuf("sxf%d" % i, [128, 1024], F32) for i in range(2)]
        W.sq = [P.sbuf("ssq%d" % i, [128, 1024], F32) for i in range(2)]
        W.t1 = [P.sbuf("st1%d" % i, [128, 1024], F32) for i in range(2)]
        W.nst = [P.sbuf("snst%d" % i, [128, 32], F32) for i in range(2)]
        qnw = P.sbuf("sqnw", [128, 64], F32)
        knw = P.sbuf("sknw", [128, 64], F32)
        P.dma("sp", qnw[:], I["sw_q_norm"].v(I["sw_q_norm"].h[j].partition_broadcast(128)))
        P.dma("sp", knw[:], I["sw_k_norm"].v(I["sw_k_norm"].h[j].partition_broadcast(128)))
        esink = P.sbuf("sesink", [128, 16], F32)
        P.dma("sp", esink[:], I["sw_sink"].v(I["sw_sink"].h[j].partition_broadcast(128)))
        P.act(esink[:], esink[:], AF.Exp)
        onesb = P.sbuf("sones", [128, 128], BF16)
        P.memset("pool", onesb[:], 1.0)
        ropeb = [P.sbuf("srope%d" % i, [128, 128], F32) for i in range(2)]
        hTt = [P.sbuf("shT%d" % i, [128, 8, 128], BF16) for i in range(2)]
        M = Modulator(P, C, l, 0, nbuf=1)
        with P.scope():
            wkv = P.sbuf("swkv", [128, 8, 512], BF16)
            P.dma("pool", wkv[:], I["sw_w_qkv"].v(wq3[:, :, 1024:1536]))
            pkv = [P.psum("spkv%d" % i, [128, 512], F32) for i in range(2)]
            pkt = [P.psum("spkt%d" % i, [64, 512], BF16) for i in range(2)]
            kb16 = [P.sbuf("skb%d" % i, [128, 256], BF16) for i in range(2)]
            kTt = [P.sbuf("skT%d" % i, [64, 4, 128], BF16) for i in range(2)]
            V2t = [P.sbuf("sV2%d" % i, [128, 4, 2, 64], BF16) for i in range(2)]
            for t in range(NTILE):
                i = t % 2
                h = hTt[i]
                M.tile(t, lambda dc, h=h: h[:, dc, :])
                for dc in range(8):
                    P.matmul(pkv[i][:], h[:, dc, :], wkv[:, dc, :], start=(dc == 0), stop=(dc == 7))
                rp = None
                if t >= NT_CTX:
                    rp = ropeb[i]
                    P.dma("sp", rp[:], I["rope"].v(I["rope"].h[128 * (t - 2):128 * (t - 2) + 128, :]))
                    rp = rp[:]
                swa_normrope(P, C, W, pkv[i][:, 0:256], SW_KV, knw, rp, kb16[i][:], i)
                for g in range(SW_KV):
                    P.transpose(pkt[i][:, 128 * g:128 * g + 128], kb16[i][:, 64 * g:64 * g + 64], C.identb[:])
                P.copy("act", kTt[i].v(rr(kTt[i].h[:], "p g t -> p (g t)")), pkt[i][:])
                vsrc = pkv[i].v(rr(pkv[i].h[:, 256:512], "p (g d) -> p g d", d=64).unsqueeze(2).to_broadcast([128, 4, 2, 64]))
                P.copy("dve", V2t[i][:], vsrc)
                P.dma("sp", kTs.v(kTs.h[t]), kTt[i][:])
                P.dma("sp", V2s.v(rr(V2s.h[t], "p g (a d) -> p g a d", a=2)), V2t[i][:])
                if t == NTILE - 1:
                    sk, rk, sv, rv = D["sndk"], D["rcvk"], D["sndv"], D["rcvv"]
                    P.dma("sp", sk.v(rr(sk.h, "p (g t) -> p g t", g=4)), kTt[i][:])
                    P.dma("sp", sv.v(rr(sv.h, "p (g a d) -> p g a d", g=4, a=2)), V2t[i][:])
                    for (s_, r_) in ((sk, rk), (sv, rv)):
                        sh_, rh_ = s_.h, r_.h
                        P.dma("pool", r_.v(rh_), s_.v(sh_), inc=1,
                              emit=lambda e, sh_=sh_, rh_=rh_: e.collective_compute("AllGather", ALU.bypass, replica_groups=GROUPS,
                                                                                    ins=[sh_.opt()], outs=[rh_.opt()]))
                    hk = [P.sbuf("shk%d" % q, [64, 512], BF16) for q in range(2)]
                    hv = [P.sbuf("shv%d" % q, [128, 512], BF16) for q in range(2)]
                    for q in range(2):
                        P.dma("sp", hk[q][:], rk.v(rk.h[64 * q:64 * q + 64, :]))
                        P.dma("sp", hv[q][:], rv.v(rv.h[128 * q:128 * q + 128, :]))
                    P.ts("dve", hk[0][:], hk[0][:], C.sel[0:64, 0:1], ALU.mult)
                    P.stt("dve", hk[0][:], hk[1][:], C.sel[0:64, 1:2], hk[0][:], ALU.mult, ALU.add)
                    P.ts("dve", hv[0][:], hv[0][:], C.sel[:, 0:1], ALU.mult)
                    P.stt("dve", hv[0][:], hv[1][:], C.sel[:, 1:2], hv[0][:], ALU.mult, ALU.add)
                    P.dma("sp", kTs.v(rr(kTs.h[HALO], "p g t -> p (g t)")), hk[0][:])
                    P.dma("sp", V2s.v(rr(V2s.h[HALO], "p g d -> p (g d)")), hv[0][:])
        with P.scope():
            wq = P.sbuf("swq", [128, 8, 1024], BF16)
            P.dma("pool", wq[:], I["sw_w_qkv"].v(wq3[:, :, 0:1024]))
            wo = P.sbuf("swo", [128, 8, 1024], BF16)
            P.dma("pool", wo[:], I["sw_w_out"].v(rr(I["sw_w_out"].h[j], "(c p) f -> p c f", p=128)))
            gb = [load_gbc(P, C, l, 0, 0, "sgb0")]
            if ctx_live:
                gb.append(load_gbc(P, C, l, 0, 1, "sgb1"))
            mask_h = P.sbuf("smaskh", [128, 128], F32)
            P.memset("pool", mask_h[:], 1.0)
            mh = mask_h.h
            P.op("pool", lambda e: e.affine_select(out=mh[:], in_=mh[:], pattern=[[1, 128]], compare_op=ALU.is_ge,
                                                   fill=0.0, base=-127, channel_multiplier=1), [mask_h], [mask_h])
            kslot = [P.sbuf("sks%d" % i, [64, 4, 128], BF16) for i in range(6)]
            vslot = [P.sbuf("svs%d" % i, [128, 4, 128], BF16) for i in range(6)]
            loaded = {}

            def key_tile(kt):
                s = kt if kt < 2 else 2 + (kt % 4)
                if loaded.get(s) != kt:
                    P.dma("sp", kslot[s][:], kTs.v(kTs.h[kt]))
                    P.dma("sp", vslot[s][:], V2s.v(V2s.h[kt]))
                    loaded[s] = kt
                return kslot[s], vslot[s]

            pq = [P.psum("spq%d" % i, [128, 512], F32) for i in range(2)]
            pqt_ap = M.pt[0].h[0:64, :].bitcast(BF16)
            psc = [P.psum("spsc%d" % i, [128, 512], F32) for i in range(2)]
            pso = P.psum("spso", [128, 512], F32)
            psd = P.psum("spsd", [128, 512], F32)
            qb16 = [P.sbuf("sqb%d" % i, [128, 1024], BF16) for i in range(2)]
            qT = [P.sbuf("sqT%d" % i, [64, 16, 128], BF16) for i in range(2)]
            PT = [P.sbuf("sPT%d" % i, [128, 512], BF16) for i in range(6)]
            den = [P.sbuf("sden%d" % i, [128, 512], F32) for i in range(2)]
            aT = [P.sbuf("saT%d" % i, [128, 8, 128], BF16) for i in range(2)]
            tmp = [P.sbuf("stmp%d" % i, [128, 512], F32) for i in range(2)]
            qtiles = list(range(NT_CTX, NTILE)) + ([0, 1] if ctx_live else [])
            kpt = 0
            ky = 0
            for n_, t in enumerate(qtiles):
                i = n_ % 2
                r = 1 if t < NT_CTX else 0
                h = hTt[i]
                M.tile(t, lambda dc, h=h: h[:, dc, :])
                for hf in range(2):
                    for dc in range(8):
                        P.matmul(pq[hf][:], h[:, dc, :], wq[:, dc, 512 * hf:512 * hf + 512], start=(dc == 0), stop=(dc == 7))
                rp = None
                if r == 0:
                    rpb = ropeb[i]
                    P.dma("sp", rpb[:], I["rope"].v(I["rope"].h[128 * (t - 2):128 * (t - 2) + 128, :]))
                    rp = rpb[:]
                for hf in range(2):
                    swa_normrope(P, C, W, pq[hf][:], 8, qnw, rp, qb16[i][:, 512 * hf:512 * hf + 512], hf)
                for hh in range(SW_H):
                    P.transpose(M.pt[0].v(pqt_ap[:, 128 * hh:128 * hh + 128]), qb16[i][:, 64 * hh:64 * hh + 64], C.identb[:])
                P.copy("act", qT[i].v(rr(qT[i].h[:, 0:8], "p h t -> p (h t)")), M.pt[0].v(pqt_ap[:, 0:1024]))
                P.copy("dve", qT[i].v(rr(qT[i].h[:, 8:16], "p h t -> p (h t)")), M.pt[0].v(pqt_ap[:, 1024:2048]))
                if r == 1:
                    kbs = [(0, None), (1, None)]
                else:
                    kbs = []
                    if t - 1 >= NT_CTX:
                        kbs.append((t - 1, C.mask_b))
                    kbs.append((t, None))
                    if t + 1 < NTILE:
                        kbs.append((t + 1, C.mask_f))
                    else:
                        kbs.append((HALO, mask_h))
                    kbs += [(0, None), (1, None)]
                kv = [key_tile(kt) for kt, _ in kbs]
                for g in range(SW_KV):
                    pts = []
                    for bi, (kt, msk) in enumerate(kbs):
                        ps = psc[kpt % 2]
                        pt_ = PT[kpt % 6]
                        kpt += 1
                        P.matmul(ps[:], kv[bi][0][:, g, :], qT[i].v(rr(qT[i].h[:, 4 * g:4 * g + 4, :], "p h t -> p (h t)")))
                        P.act(pt_[:], ps[:], AF.Exp, scale=0.125)
                        if msk is not None:
                            p3 = rr(pt_.h[:], "p (h t) -> p h t", t=128)
                            P.tt("dve", pt_.v(p3), pt_.v(p3), msk.v(msk.h[:].unsqueeze(1).to_broadcast([128, 4, 128])), ALU.mult)
                        pts.append(pt_)
                    nb = len(kbs)
                    for bi in range(nb):
                        P.matmul(pso[:], kv[bi][1][:, g, :], pts[bi][:], start=(bi == 0), stop=(bi == nb - 1))
                    for bi in range(nb):
                        P.matmul(psd[:], onesb[:], pts[bi][:], start=(bi == 0), stop=(bi == nb - 1))
                    dn = den[g % 2]
                    d3 = rr(dn.h[:], "p (h t) -> p h t", t=128)
                    P.tt("dve", dn.v(d3), psd.v(rr(psd.h[:], "p (h t) -> p h t", t=128)),
                         esink.v(esink.h[:, 4 * g:4 * g + 4].unsqueeze(2).to_broadcast([128, 4, 128])), ALU.add)
                    P.recip(dn[:], dn[:])
                    for half in range(2):
                        rows = slice(64 * half, 64 * half + 64)
                        o_v = aT[i].v(aT[i].h[rows, 2 * g:2 * g + 2, :])
                        src = pso.v(rr(pso.h[rows, :], "p (a b t) -> p a b t", a=2, b=2)[:, :, half, :])
                        dsrc = dn.v(rr(dn.h[rows, :], "p (a b t) -> p a b t", a=2, b=2)[:, :, half, :])
                        P.tt("dve", o_v, src, dsrc, ALU.mult)
                for hf in range(2):
                    p = pq[hf]
                    tm = tmp[ky % 2]
                    ky += 1
                    for c in range(8):
                        P.matmul(p[:], aT[i][:, c, :], wo[:, c, 512 * hf:512 * hf + 512], start=(c == 0), stop=(c == 7))
                    P.tt("dve", tm[:], p[:], gb[r][:, 512 * hf:512 * hf + 512], ALU.mult)
                    xs = C.X[t][:, 512 * hf:512 * hf + 512]
                    P.tt("pool", xs, xs, tm[:], ALU.add)


def build(nl=4, dbg=True):
    nc = bass.Bass("TRN2", target_bir_lowering=False)
    P = Prog(nc)
    C = Consts()
    I = {}

    def inp(name, shape):
        I[name] = P.dram(name, shape, F32, kind="ExternalInput")

    inp("x_in", [2048, 1024])
    inp("ctx_in", [256, 1024])
    inp("cvec", [2, 1024])
    inp("sel", [128, 2])
    inp("w_mod", [4, 1024, 6144])
    inp("b_mod", [4, 6144])
    inp("hg_w_in", [2, 1024, 5120])
    inp("hg_lb", [2, 2, 1024])
    inp("hg_norm_w", [2, 128])
    inp("hg_w_out", [2, 1024, 1024])
    inp("ff_w_gate_up", [2, 1024, 2 * D_FF])
    inp("ff_w_down", [2, D_FF, 1024])
    if nl > 1:
        inp("sw_w_qkv", [2, 1024, 1536])
        inp("sw_q_norm", [2, 64])
        inp("sw_k_norm", [2, 64])
        inp("sw_sink", [2, 16])
        inp("sw_w_out", [2, 1024, 1024])
        inp("rope", [2048, 128])
    if (nl > 2 or (nl == 2 and dbg != "mid")) and dbg != "nomoe":
        inp("moe_router", [2, 1024, 8])
        inp("moe_gu", [2, 1024, 2 * D_EXPERT])
        inp("moe_dn", [2, D_EXPERT, 1024])
    y = P.dram("y", [2048, 1024], F32, kind="ExternalOutput")
    yc = P.dram("yc", [256, 1024], F32, kind="ExternalOutput")
    D = {}
    D["qS"] = P.dram("qS", [8, 128, NTOK], BF16)
    D["ktS"] = P.dram("ktS", [2, 8, 128, NTOK], BF16)
    D["lfS"] = P.dram("lfS", [2, 8, 128, NTOK], F32)
    D["vS"] = P.dram("vS", [NTOK, 1024], BF16)
    D["gS"] = P.dram("gS", [NTOK, 1024], BF16)
    D["oS"] = P.dram("oS", [NTOK, 1024], F32)
    D["kTs"] = P.dram("kTs", [NKT, 64, 4, 128], BF16)
    D["V2s"] = P.dram("V2s", [NKT, 128, 4, 128], BF16)
    D["sndk"] = P.dram("sndk", [64, 512], BF16)
    D["rcvk"] = P.dram("rcvk", [128, 512], BF16)
    D["sndv"] = P.dram("sndv", [128, 512], BF16)
    D["rcvv"] = P.dram("rcvv", [256, 512], BF16)
    D["snd"] = P.dram("snd", [1024, 128], F32)
    D["rcv"] = P.dram("rcv", [2048, 128], F32)

    moe_on = "moe_gu" in I
    if moe_on:
        ALLG = [list(range(8))]
        Ggu, Gdn = [], []
        for l_ in range(2):
            for nm, rows, cols, lst in (("gu", 1024, 2 * D_EXPERT, Ggu), ("dn", D_EXPERT, 1024, Gdn)):
                s_ = P.dram("s%s%d" % (nm, l_), [rows, cols], F32)
                g_ = P.dram("G%s%d" % (nm, l_), [8 * rows, cols], F32)
                src = I["moe_" + nm]
                for r0 in range(0, rows, 128):
                    P.dma("sp", s_.v(s_.h[r0:r0 + 128, :]), src.v(src.h[l_, r0:r0 + 128, :]))
                sh_, gh_ = s_.h, g_.h
                P.dma("pool", g_.v(gh_), s_.v(sh_), inc=1,
                      emit=lambda e, sh_=sh_, gh_=gh_: e.collective_compute("AllGather", ALU.bypass, replica_groups=ALLG,
                                                                            ins=[sh_.opt()], outs=[gh_.opt()]))
                lst.append(g_)
    make_consts(P, C)
    C.epscol = P.sbuf("epscol", [128, 1], F32)
    P.memset("pool", C.epscol[:], EPS)
    C.sel = P.sbuf("sel", [128, 2], F32)
    P.dma("sp", C.sel[:], I["sel"].v(I["sel"].h))
    C.X = [P.sbuf("X%d" % t, [128, 1024], F32) for t in range(NTILE)]
    for t in range(NTILE):
        if t < NT_CTX:
            P.dma("sp", C.X[t][:], I["ctx_in"].v(I["ctx_in"].h[128 * t:128 * t + 128, :]))
        else:
            P.dma("sp", C.X[t][:], I["x_in"].v(I["x_in"].h[128 * (t - 2):128 * (t - 2) + 128, :]))
    stage_mod(P, C, I)
    stage_lb(P, C, I)
    for l in range(nl):
        j = l // 2
        ctx_live = l < 3
        tiles_f = list(range(NTILE)) if ctx_live else list(range(NT_CTX, NTILE))
        if l % 2 == 0:
            with P.scope():
                hT = P.sbuf("hT", [128, 8, NTOK], BF16)
                stage_modulate(P, C, l, 0, list(range(NTILE)), hT)
                stage_hg_proj(P, C, j, hT, I, D)
            stage_hg_scan(P, C, j, I, D)
            stage_hg_readout(P, C, l, j, I, D)
            if dbg == "mid" and l == nl - 1:
                break
            with P.scope():
                hT = P.sbuf("hT", [128, 8, NTOK], BF16)
                stage_modulate(P, C, l, 1, tiles_f, hT)
                stage_ffn(P, C, l, hT, tiles_f,
                          [(I["ff_w_gate_up"], I["ff_w_gate_up"].h[j], I["ff_w_down"], I["ff_w_down"].h[j], D_FF, None)])
        else:
            stage_swa(P, C, l, j, I, D, ctx_live)
            if dbg == "mid" and l == nl - 1:
                break
            if dbg == "nomoe":
                continue
            with P.scope():
                hT = P.sbuf("hT", [128, 8, NTOK], BF16)
                rw = P.sbuf("rtw", [128, 8, 8], F32)
                P.dma("sp", rw[:], I["moe_router"].v(rr(I["moe_router"].h[j], "(c p) e -> p c e", p=128)))
                gates = P.sbuf("gates", [128, NTILE, 8], F32)
                stage_modulate(P, C, l, 1, tiles_f, hT, router=dict(w=rw, gates=gates))
                specs = [(Ggu[j], Ggu[j].h[1024 * e:1024 * e + 1024, :], Gdn[j], Gdn[j].h[D_EXPERT * e:D_EXPERT * e + D_EXPERT, :], D_EXPERT, e)
                         for e in range(N_EXPERTS)]
                stage_ffn(P, C, l, hT, tiles_f, specs, gates=gates)
    for t in range(NTILE):
        if t < NT_CTX:
            P.dma("sp", yc.v(yc.h[128 * t:128 * t + 128, :]), C.X[t][:])
        else:
            P.dma("sp", y.v(y.h[128 * (t - 2):128 * (t - 2) + 128, :]), C.X[t][:])
    P.finish()
    print("ops", P.stats, "dsems", len(P.dsems))
    return nc


def rope_table(pos):
    row = (pos // 64).astype(np.float32)
    col = (pos % 64).astype(np.float32)
    inv = (10000.0 ** (-np.arange(16, dtype=np.float32) / 16)).astype(np.float32)
    ar = row[:, None] * inv[None, :]
    ac = col[:, None] * inv[None, :]
    cc = np.concatenate([np.cos(ar), np.cos(ar), np.cos(ac), np.cos(ac)], axis=1)
    ss = np.concatenate([-np.sin(ar), np.sin(ar), -np.sin(ac), np.sin(ac)], axis=1)
    return np.ascontiguousarray(np.concatenate([cc, ss], axis=1), dtype=np.float32)


def prep_inputs(inputs, nl=4, dbg=True):
    f = lambda a: np.ascontiguousarray(a, dtype=np.float32)
    maps = []
    for c in range(8):
        b, hf = c // 2, c % 2
        m = {}
        xl = inputs["x"][b, hf * 2048:(hf + 1) * 2048]
        cx = inputs["ctx"][b]
        win = inputs["hg_w_in"]
        lb = inputs["hg_lb_logits"]
        if hf == 1:
            xl = xl[::-1]
            cx = cx[::-1]
            win = np.concatenate([win[:, :, 0:2048], win[:, :, 3072:4096], win[:, :, 2048:3072], win[:, :, 4096:5120]], axis=2)
            lb = lb[:, ::-1]
        m["x_in"] = f(xl)
        m["ctx_in"] = f(cx)
        m["cvec"] = f(np.stack([inputs["c"][b], inputs["c_ctx"]]))
        sel = np.zeros((128, 2), np.float32)
        sel[:, 1 - hf] = 1.0
        m["sel"] = sel
        m["w_mod"] = f(inputs["w_mod"])
        m["b_mod"] = f(inputs["b_mod"])
        m["hg_w_in"] = f(win)
        m["hg_lb"] = f(lb)
        m["hg_norm_w"] = f(inputs["hg_norm_w"])
        m["hg_w_out"] = f(inputs["hg_w_out"])
        m["ff_w_gate_up"] = f(inputs["ff_w_gate_up"])
        m["ff_w_down"] = f(inputs["ff_w_down"])
        if nl > 1:
            for k_ in ("sw_w_qkv", "sw_q_norm", "sw_k_norm", "sw_sink", "sw_w_out"):
                m[k_] = f(inputs[k_])
            pos = np.arange(2048) + hf * 2048
            if hf == 1:
                pos = pos[::-1]
            m["rope"] = rope_table(pos)
        if (nl > 2 or (nl == 2 and dbg != "mid")) and dbg != "nomoe":
            m["moe_router"] = f(inputs["moe_router"])
            m["moe_gu"] = f(inputs["moe_w_gate_up"][:, c])
            m["moe_dn"] = f(inputs["moe_w_down"][:, c])
        maps.append(m)
    return maps


def gather(results):
    out = np.zeros((4, 4096, 1024), np.float32)
    ctx = np.zeros((4, 2, 256, 1024), np.float32)
    for c in range(8):
        b, hf = c // 2, c % 2
        y = results[c]["y"]
        yc = results[c]["yc"]
        if hf == 1:
            y = y[::-1]
            yc = yc[::-1]
        out[b, hf * 2048:(hf + 1) * 2048] = y
        ctx[b, hf] = yc
    return out, ctx


def kernel(**inputs):
    maps = prep_inputs(inputs, 4, False)
    nc = build(4, False)
    res = run_bass_kernel_spmd(nc, maps, core_ids=list(range(8)))
    out, _ = gather(res.results)
    return out
```
